# Optimizing a Trainium2 kernel written in Bass

```python
import math
import jax
import jax.numpy as jnp
from jax import lax
import numpy as np

D_MODEL = 1024
BATCH = 4
SEQ = 8192
DEPTH = 2

CHUNK = 64
CONV_K = 4
GDN_HEADS = D_MODEL // 128
GDN_DK = 128
GDN_DV = 128
GDN_QK = GDN_HEADS * GDN_DK
GDN_V = GDN_HEADS * GDN_DV
GDN_CONV_CH = 2 * GDN_QK + GDN_V
SSD_P = 64
SSD_HEADS = D_MODEL // SSD_P
SSD_G = 2
SSD_N = 128
SSD_INNER = SSD_HEADS * SSD_P
SSD_BC = SSD_G * SSD_N
SSD_CONV_CH = SSD_INNER + 2 * SSD_BC
MIX_WIDTH = GDN_V + SSD_INNER
IN_WIDTH = GDN_CONV_CH + GDN_V + 2 * GDN_HEADS + SSD_INNER + SSD_CONV_CH + SSD_HEADS
N_EXPERTS = 32
TOP_K = 4
D_FF = D_MODEL
SWIGLU_ALPHA = 1.702
SWIGLU_LIMIT = 7.0
EXPERT_BLOCK = 128
EPS = 1e-6

kernel_name = "hybrid_gdn_ssd_moe_adaln_block"


def rmsnorm(x, g):
    xf = x.astype(jnp.float32)
    y = xf * lax.rsqrt(jnp.mean(xf * xf, axis=-1, keepdims=True) + EPS)
    return (y * g.astype(jnp.float32)).astype(x.dtype)


def l2norm(x):
    return x * lax.rsqrt(jnp.sum(x * x, axis=-1, keepdims=True) + EPS)


def causal_dwconv(x, w):
    k, ch = w.shape
    return lax.conv_general_dilated(x, w[:, None, :], window_strides=(1,), padding=[(k - 1, 0)],
                                    dimension_numbers=("NWC", "WIO", "NWC"), feature_group_count=ch)


def gated_delta_rule_chunked(q, k, v, beta, g):
    bsz, s, h, dk = q.shape
    dv = v.shape[-1]
    nc = s // CHUNK

    def blocks(t):
        return jnp.moveaxis(t.reshape((bsz, nc, CHUNK, h) + t.shape[3:]), 3, 1)

    qc, kc, vc, bc, gc = blocks(q), blocks(k), blocks(v), blocks(beta), blocks(g)
    gam = jnp.cumsum(gc, axis=-1)
    incl = jnp.tril(jnp.ones((CHUNK, CHUNK), bool))
    strict = jnp.tril(jnp.ones((CHUNK, CHUNK), bool), -1)
    decay = jnp.exp(jnp.where(incl, gam[..., :, None] - gam[..., None, :], -jnp.inf))
    kk = jnp.einsum("bhnld,bhnsd->bhnls", kc, kc)
    a_mat = jnp.where(strict, bc[..., :, None] * kk * decay, 0.0) + jnp.eye(CHUNK, dtype=q.dtype)
    rhs = jnp.concatenate([vc * bc[..., None], kc * (bc * jnp.exp(gam))[..., None]], axis=-1)
    sol = lax.linalg.triangular_solve(a_mat, rhs, left_side=True, lower=True, unit_diagonal=True)
    u, w = sol[..., :dv], sol[..., dv:]
    qk = jnp.einsum("bhnld,bhnsd->bhnls", qc, kc) * decay
    q_dec = qc * jnp.exp(gam)[..., None]
    k_dec = kc * jnp.exp(gam[..., -1:] - gam)[..., None]
    chunk_dec = jnp.exp(gam[..., -1])

    def step(state, inp):
        u_i, w_i, qk_i, qd_i, kd_i, cd_i = inp
        v_new = u_i - jnp.einsum("bhld,bhdv->bhlv", w_i, state)
        o_i = jnp.einsum("bhld,bhdv->bhlv", qd_i, state) + jnp.einsum("bhls,bhsv->bhlv", qk_i, v_new)
        state = cd_i[..., None, None] * state + jnp.einsum("bhld,bhlv->bhdv", kd_i, v_new)
        return state, o_i

    xs = (jnp.moveaxis(u, 2, 0), jnp.moveaxis(w, 2, 0), jnp.moveaxis(qk, 2, 0),
          jnp.moveaxis(q_dec, 2, 0), jnp.moveaxis(k_dec, 2, 0), jnp.moveaxis(chunk_dec, 2, 0))
    s0 = jnp.zeros((bsz, h, dk, dv), q.dtype)
    _, o = lax.scan(step, s0, xs)
    return jnp.transpose(o, (1, 0, 3, 2, 4)).reshape(bsz, s, h, dv)


def ssd_chunked(x, dt, a_log, bm, cm):
    bsz, s, h, p = x.shape
    g, n = bm.shape[2], bm.shape[3]
    e = h // g
    nc = s // CHUNK
    a = -jnp.exp(a_log) * dt
    xc = (x * dt[..., None]).reshape(bsz, nc, CHUNK, g, e, p)
    ac = a.reshape(bsz, nc, CHUNK, g, e)
    bc = bm.reshape(bsz, nc, CHUNK, g, n)
    cc = cm.reshape(bsz, nc, CHUNK, g, n)
    a_cum = jnp.cumsum(ac, axis=2)
    incl = jnp.tril(jnp.ones((CHUNK, CHUNK), bool))
    seg = a_cum[:, :, :, None] - a_cum[:, :, None, :]
    decay = jnp.exp(jnp.where(incl[:, :, None, None], seg, -jnp.inf))
    cb = jnp.einsum("bclgn,bcsgn->bclsg", cc, bc)
    y_diag = jnp.einsum("bclsge,bcsgep->bclgep", cb[..., None] * decay, xc)
    states = jnp.einsum("bclgn,bclge,bclgep->bcgepn", bc, jnp.exp(a_cum[:, :, -1:] - a_cum), xc)
    chunk_dec = jnp.exp(a_cum[:, :, -1])

    def step(hs, inp):
        st, cd = inp
        return cd[..., None, None] * hs + st, hs

    h0 = jnp.zeros((bsz, g, e, p, n), x.dtype)
    _, h_prev = lax.scan(step, h0, (jnp.moveaxis(states, 1, 0), jnp.moveaxis(chunk_dec, 1, 0)))
    h_prev = jnp.moveaxis(h_prev, 0, 1)
    y_off = jnp.einsum("bclgn,bcgepn,bclge->bclgep", cc, h_prev, jnp.exp(a_cum))
    return (y_diag + y_off).reshape(bsz, s, h, p)


def hybrid_mixer(h, w_in, gdn_conv_w, gdn_a_log, gdn_dt_bias, gdn_norm_g,
                 ssd_conv_w, ssd_conv_b, ssd_a_log, ssd_dt_bias, ssd_d, ssd_norm_g, w_out):
    bsz, s, _ = h.shape
    f32 = jnp.float32
    proj = h @ w_in
    off = 0
    gdn_qkv = proj[..., off:off + GDN_CONV_CH]; off += GDN_CONV_CH
    gdn_z = proj[..., off:off + GDN_V]; off += GDN_V
    gdn_b = proj[..., off:off + GDN_HEADS]; off += GDN_HEADS
    gdn_a = proj[..., off:off + GDN_HEADS]; off += GDN_HEADS
    ssd_z = proj[..., off:off + SSD_INNER]; off += SSD_INNER
    ssd_xbc = proj[..., off:off + SSD_CONV_CH]; off += SSD_CONV_CH
    ssd_dt = proj[..., off:off + SSD_HEADS]

    qkv = jax.nn.silu(causal_dwconv(gdn_qkv, gdn_conv_w)).astype(f32)
    q = l2norm(qkv[..., :GDN_QK].reshape(bsz, s, GDN_HEADS, GDN_DK)) * (GDN_DK ** -0.5)
    k = l2norm(qkv[..., GDN_QK:2 * GDN_QK].reshape(bsz, s, GDN_HEADS, GDN_DK))
    v = qkv[..., 2 * GDN_QK:].reshape(bsz, s, GDN_HEADS, GDN_DV)
    beta = jax.nn.sigmoid(gdn_b.astype(f32))
    g = -jnp.exp(gdn_a_log.astype(f32)) * jax.nn.softplus(gdn_a.astype(f32) + gdn_dt_bias.astype(f32))
    o = gated_delta_rule_chunked(q, k, v, beta, g)
    o = rmsnorm(o, gdn_norm_g) * jax.nn.silu(gdn_z.astype(f32).reshape(bsz, s, GDN_HEADS, GDN_DV))
    gdn_out = o.reshape(bsz, s, GDN_V).astype(h.dtype)

    xbc = jax.nn.silu(causal_dwconv(ssd_xbc, ssd_conv_w) + ssd_conv_b).astype(f32)
    xs = xbc[..., :SSD_INNER].reshape(bsz, s, SSD_HEADS, SSD_P)
    bm = xbc[..., SSD_INNER:SSD_INNER + SSD_BC].reshape(bsz, s, SSD_G, SSD_N)
    cm = xbc[..., SSD_INNER + SSD_BC:].reshape(bsz, s, SSD_G, SSD_N)
    dt = jax.nn.softplus(ssd_dt.astype(f32) + ssd_dt_bias.astype(f32))
    y = ssd_chunked(xs, dt, ssd_a_log.astype(f32), bm, cm) + ssd_d.astype(f32)[:, None] * xs
    y = y.reshape(bsz, s, SSD_INNER) * jax.nn.silu(ssd_z.astype(f32))
    y = rmsnorm(y.reshape(bsz, s, SSD_G, SSD_INNER // SSD_G),
                ssd_norm_g.reshape(SSD_G, SSD_INNER // SSD_G)).reshape(bsz, s, SSD_INNER)
    ssd_out = y.astype(h.dtype)

    return jnp.concatenate([gdn_out, ssd_out], axis=-1) @ w_out


def clamped_swiglu(gu):
    gate, up = gu[..., :D_FF], gu[..., D_FF:]
    gate = jnp.minimum(gate, SWIGLU_LIMIT)
    up = jnp.clip(up, -SWIGLU_LIMIT, SWIGLU_LIMIT)
    return (up + 1.0) * (gate * jax.nn.sigmoid(gate * SWIGLU_ALPHA))


def moe_ffn(h, router_w, router_b, w_gu, b_gu, w_down, b_down):
    bsz, s, d = h.shape
    n_tok = bsz * s
    hf = h.reshape(n_tok, d)
    logits = (hf @ router_w + router_b).astype(jnp.float32)
    top_logit, top_idx = lax.top_k(logits, TOP_K)
    gates = jax.nn.softmax(top_logit, axis=-1)
    n_assign = n_tok * TOP_K
    eid = top_idx.reshape(n_assign)
    tok = jnp.arange(n_assign, dtype=jnp.int32) // TOP_K
    order = jnp.argsort(eid)
    eid_s = eid[order]
    counts = jnp.bincount(eid, length=N_EXPERTS)
    padded = (counts + EXPERT_BLOCK - 1) // EXPERT_BLOCK * EXPERT_BLOCK
    pad_end = jnp.cumsum(padded)
    pad_start = pad_end - padded
    start = jnp.cumsum(counts) - counts
    dest = pad_start[eid_s] + jnp.arange(n_assign, dtype=jnp.int32) - start[eid_s]
    n_rows = (-(-n_assign // EXPERT_BLOCK) + N_EXPERTS) * EXPERT_BLOCK
    n_blocks = n_rows // EXPERT_BLOCK
    row_tok = jnp.full((n_rows,), n_tok, jnp.int32).at[dest].set(tok[order])
    row_gate = jnp.zeros((n_rows,), jnp.float32).at[dest].set(gates.reshape(n_assign)[order])
    blk_start = jnp.arange(n_blocks, dtype=jnp.int32) * EXPERT_BLOCK
    blk_expert = jnp.minimum(jnp.searchsorted(pad_end, blk_start, side="right"), N_EXPERTS - 1)
    h_pad = jnp.concatenate([hf, jnp.zeros((1, d), hf.dtype)], axis=0)
    x_rows = h_pad[row_tok].reshape(n_blocks, EXPERT_BLOCK, d)

    def expert_block(args):
        xb, e = args
        y = clamped_swiglu(xb @ w_gu[e] + b_gu[e])
        return y @ w_down[e] + b_down[e]

    y_rows = lax.map(expert_block, (x_rows, blk_expert)).reshape(n_rows, d)
    y_rows = y_rows * row_gate[:, None].astype(y_rows.dtype)
    out = jax.ops.segment_sum(y_rows, row_tok, num_segments=n_tok + 1)[:n_tok]
    return out.reshape(bsz, s, d)


def setup_inputs(seed: int = 0) -> dict:
    key = jax.random.key(seed)
    ks = jax.random.split(key, 26)
    f32 = jnp.float32
    L = DEPTH

    def nrm(k, shape, scale):
        return jax.random.normal(k, shape, f32) * scale

    def a_log_init(k, h):
        return jnp.log(jax.random.uniform(k, (L, h), f32, minval=1.0, maxval=16.0))

    def dt_bias_init(k, h):
        dt = jnp.exp(jax.random.uniform(k, (L, h), f32, minval=math.log(1e-3), maxval=math.log(1e-1)))
        return dt + jnp.log(-jnp.expm1(-dt))

    return {
        "x": nrm(ks[0], (BATCH, SEQ, D_MODEL), 1.0),
        "c": nrm(ks[1], (BATCH, D_MODEL), 1.0),
        "ada_w": nrm(ks[2], (L, D_MODEL, 6 * D_MODEL), 0.5 * D_MODEL ** -0.5),
        "ada_b": nrm(ks[3], (L, 6 * D_MODEL), 0.02),
        "norm1_g": 1.0 + nrm(ks[4], (L, D_MODEL), 0.02),
        "norm2_g": 1.0 + nrm(ks[5], (L, D_MODEL), 0.02),
        "w_in": nrm(ks[6], (L, D_MODEL, IN_WIDTH), D_MODEL ** -0.5),
        "gdn_conv_w": nrm(ks[7], (L, CONV_K, GDN_CONV_CH), CONV_K ** -0.5),
        "gdn_a_log": a_log_init(ks[8], GDN_HEADS),
        "gdn_dt_bias": dt_bias_init(ks[9], GDN_HEADS),
        "gdn_norm_g": 1.0 + nrm(ks[10], (L, GDN_DV), 0.02),
        "ssd_conv_w": nrm(ks[11], (L, CONV_K, SSD_CONV_CH), CONV_K ** -0.5),
        "ssd_conv_b": nrm(ks[12], (L, SSD_CONV_CH), 0.02),
        "ssd_a_log": a_log_init(ks[13], SSD_HEADS),
        "ssd_dt_bias": dt_bias_init(ks[14], SSD_HEADS),
        "ssd_d": 1.0 + nrm(ks[15], (L, SSD_HEADS), 0.02),
        "ssd_norm_g": 1.0 + nrm(ks[16], (L, SSD_INNER), 0.02),
        "w_out": nrm(ks[17], (L, MIX_WIDTH, D_MODEL), MIX_WIDTH ** -0.5),
        "router_w": nrm(ks[18], (L, D_MODEL, N_EXPERTS), D_MODEL ** -0.5),
        "router_b": nrm(ks[19], (L, N_EXPERTS), 0.01),
        "moe_w_gu": nrm(ks[20], (L, N_EXPERTS, D_MODEL, 2 * D_FF), D_MODEL ** -0.5),
        "moe_b_gu": nrm(ks[21], (L, N_EXPERTS, 2 * D_FF), 0.01),
        "moe_w_down": nrm(ks[22], (L, N_EXPERTS, D_FF, D_MODEL), D_FF ** -0.5),
        "moe_b_down": nrm(ks[23], (L, N_EXPERTS, D_MODEL), 0.01),
        "final_g": 1.0 + nrm(ks[24], (D_MODEL,), 0.02),
    }


def reference(x, c, ada_w, ada_b, norm1_g, norm2_g, w_in, gdn_conv_w, gdn_a_log, gdn_dt_bias,
              gdn_norm_g, ssd_conv_w, ssd_conv_b, ssd_a_log, ssd_dt_bias, ssd_d, ssd_norm_g, w_out,
              router_w, router_b, moe_w_gu, moe_b_gu, moe_w_down, moe_b_down, final_g):
    c_act = jax.nn.silu(c)
    for l in range(DEPTH):
        mod = (c_act @ ada_w[l] + ada_b[l])[:, None, :]
        sh1, sc1, g1, sh2, sc2, g2 = jnp.split(mod, 6, axis=-1)
        h = rmsnorm(x, norm1_g[l]) * (1.0 + sc1) + sh1
        x = x + g1 * hybrid_mixer(h, w_in[l], gdn_conv_w[l], gdn_a_log[l], gdn_dt_bias[l], gdn_norm_g[l],
                                  ssd_conv_w[l], ssd_conv_b[l], ssd_a_log[l], ssd_dt_bias[l], ssd_d[l],
                                  ssd_norm_g[l], w_out[l])
        h = rmsnorm(x, norm2_g[l]) * (1.0 + sc2) + sh2
        x = x + g2 * moe_ffn(h, router_w[l], router_b[l], moe_w_gu[l], moe_b_gu[l],
                             moe_w_down[l], moe_b_down[l])
    return rmsnorm(x, final_g)
```

```python
import sys
from contextlib import ExitStack
import numpy as np
import ml_dtypes
import concourse.bass as bass
import concourse.mybir as mybir
from concourse.bass_utils import run_bass_kernel_spmd

F32 = mybir.dt.float32
BF16 = mybir.dt.bfloat16
AF = mybir.ActivationFunctionType
ALU = mybir.AluOpType
AX = mybir.AxisListType

D = 1024
BIG = 30000.0
EPS = 1e-6
ENGS = ("pe", "act", "dve", "pool", "sp")


class Res:
    __slots__ = ("name", "last_w", "readers", "sem", "dma_total", "gen_open", "last_dma", "sb")

    def __init__(self, name):
        self.name = name
        self.last_w = []
        self.readers = []
        self.sem = None
        self.dma_total = 0
        self.gen_open = False
        self.last_dma = None
        self.sb = False


class Ins:
    __slots__ = ("eng", "fn", "deps", "signal", "is_dma", "sem", "count", "waits", "line")

    def __init__(self, eng, fn, is_dma):
        self.eng = eng
        self.fn = fn
        self.deps = []
        self.signal = False
        self.is_dma = is_dma
        self.sem = None
        self.count = None
        self.waits = None


class Sched:
    def __init__(self, nc):
        self.nc = nc
        self.streams = {e: [] for e in ENGS}
        self.all_res = []
        self.final_dmas = []

    def res(self, name):
        r = Res(name)
        self.all_res.append(r)
        return r

    def _add(self, eng, fn, r, w, is_dma=False, same_gen=False):
        ins = Ins(eng, fn, is_dma)
        fr = sys._getframe(2)
        lines = []
        while fr is not None and len(lines) < 4:
            lines.append(fr.f_lineno)
            fr = fr.f_back
        ins.line = lines
        deps = []
        for x in r:
            deps.extend(x.last_w)
        for x in w:
            if is_dma and same_gen and x.gen_open:
                assert not x.readers, f"same_gen DMA after reader on {x.name}"
            else:
                deps.extend(x.last_w)
                deps.extend(x.readers)
        semres = None
        if is_dma:
            cand = [x for x in list(w) + list(r) if x.sb]
            semres = cand[0] if cand else w[0]
            if semres.last_dma is not None:
                deps.append(semres.last_dma)
        seen = set()
        for d in deps:
            if id(d) in seen:
                continue
            seen.add(id(d))
            if d.is_dma or is_dma or d.eng != eng or eng != "pe":
                ins.deps.append(d)
        for x in r:
            x.readers.append(ins)
            x.gen_open = False
        for x in w:
            if is_dma and same_gen and x.gen_open:
                x.last_w.append(ins)
            else:
                x.last_w = [ins]
                x.readers = []
            x.gen_open = bool(is_dma)
        if is_dma:
            tgt = semres
            ins.sem = tgt
            tgt.dma_total += 16
            ins.count = tgt.dma_total
            tgt.last_dma = ins
        self.streams[eng].append(ins)
        return ins

    def op(self, eng, fn, r=(), w=()):
        return self._add(eng, fn, list(r), list(w))

    def dma(self, eng, fn, r=(), w=(), same_gen=False, final=False):
        ins = self._add(eng, fn, list(r), list(w), is_dma=True, same_gen=same_gen)
        if final:
            self.final_dmas.append(ins)
        return ins

    def barrier(self, fn):
        ins = self._add("dve", fn, [], list(self.all_res))
        dmas = [r.last_dma for r in self.all_res if r.last_dma is not None]
        for e in ("pe", "act", "pool", "sp"):
            w = Ins(e, None, False)
            w.line = []
            w.deps = [ins] + dmas
            self.streams[e].append(w)
        return ins

    def finalize(self, stack):
        nc = self.nc
        for e in ENGS:
            for ins in self.streams[e]:
                for d in ins.deps:
                    d.signal = True
        eng_sem = {e: stack.enter_context(nc.semaphore(f"sem_{e}")) for e in ENGS}
        nsem = 0
        for r in self.all_res:
            if r.dma_total > 0:
                r.sem = stack.enter_context(nc.semaphore(f"dsem_{nsem}_{r.name}"))
                nsem += 1
        self.n_dma_sems = nsem
        for e in ENGS:
            c = 0
            for ins in self.streams[e]:
                if not ins.is_dma and ins.signal:
                    c += 1
                    ins.count = c
        for e in ENGS:
            waited = {}
            for ins in self.streams[e]:
                ws = {}
                for d in ins.deps:
                    if d.is_dma:
                        key = ("d", id(d.sem))
                        sem = d.sem.sem
                    else:
                        key = ("e", d.eng)
                        sem = eng_sem[d.eng]
                    if waited.get(key, 0) >= d.count:
                        continue
                    if key not in ws or ws[key][1] < d.count:
                        ws[key] = (sem, d.count)
                for key, (sem, cnt) in ws.items():
                    waited[key] = cnt
                ins.waits = list(ws.values())
        finals = [(d.sem.sem, d.sem.dma_total) for d in self.final_dmas]
        streams = self.streams

        def replay(e, eng):
            for ins in streams[e]:
                for sem, cnt in ins.waits:
                    eng.wait_ge(sem, cnt)
                if ins.fn is None:
                    continue
                try:
                    bi = ins.fn(eng)
                except Exception:
                    print("FAILED instruction recorded at lines", ins.line, flush=True)
                    raise
                if ins.is_dma:
                    bi.then_inc(ins.sem.sem, 16)
                elif ins.signal:
                    bi.then_inc(eng_sem[e], 1)
            if e == "sp":
                done = set()
                for sem, cnt in finals:
                    if id(sem) in done:
                        continue
                    done.add(id(sem))
                    eng.wait_ge(sem, cnt)

        with nc.Block() as block:
            @block.tensor
            def _(eng):
                replay("pe", eng)

            @block.scalar
            def _(eng):
                replay("act", eng)

            @block.vector
            def _(eng):
                replay("dve", eng)

            @block.gpsimd
            def _(eng):
                replay("pool", eng)

            @block.sync
            def _(eng):
                replay("sp", eng)


class _Early(Exception):
    pass


class B:
    def __init__(self, nc, S):
        self.nc = nc
        self.S = S
        self.n = 0
        self.rr = 0
        self.k = 0
        self.rcache = {}

    def tile(self, stack, shape, dt, name=None):
        self.n += 1
        nm = f"{name or 't'}_{self.n}"
        t = stack.enter_context(self.nc.sbuf_tensor(nm, list(shape), dt))
        self.k += 1
        key = (self.k, name)
        r = self.rcache.get(key)
        if r is None:
            r = self.S.res(nm)
            r.sb = True
            self.rcache[key] = r
        return t, r

    def op(self, eng, name, r, w, *args, **kw):
        self.S.op(eng, lambda e: getattr(e, name)(*args, **kw), r, w)

    def mm(self, out, lhsT, rhs, start, stop, r, w):
        self.S.op("pe", lambda e: e.matmul(out, lhsT=lhsT, rhs=rhs, start=start, stop=stop), r, w)

    def tr(self, out, in_, ident, r, w):
        self.S.op("pe", lambda e: e.transpose(out, in_, ident), r, w)

    def dma(self, eng, out, in_, r, w, same_gen=False, final=False):
        self.S.dma(eng, lambda e: e.dma_start(out=out, in_=in_), r, w, same_gen=same_gen, final=final)

    def act(self, out, in_, func, r, w, eng="act", **kw):
        self.S.op("act", lambda e: e.activation(out=out, in_=in_, func=func, **kw), r, w)

    def alt(self, engs):
        self.rr += 1
        return engs[self.rr % len(engs)]


def bc(ap, shape, axis):
    return ap.unsqueeze(axis).to_broadcast(list(shape))


def build_layer(T, dbg=False, stage=9, NE=32, NSEG=1):
    NT, NB, NC = T // 128, T // 512, T // 64
    nc = bass.Bass("TRN2", target_bir_lowering=False)

    def din(name, shape, dt=F32):
        return nc.dram_tensor(name, list(shape), dt, kind="ExternalInput")

    def dout(name, shape, dt=F32):
        return nc.dram_tensor(name, list(shape), dt, kind="ExternalOutput")

    x_in = din("x", [NSEG * T, D])
    cvec = din("cvec", [NSEG, 128, 8])
    find = din("fin", [128, 1])
    ada_w = din("ada_w", [D, 6144])
    ada_b = din("ada_b", [1, 6144])
    n1g = din("n1g", [128, D])
    n2g = din("n2g", [128, D])
    fgd = din("fg", [128, D])
    w_cm = din("w_cm", [D, 4608])
    w_tm = din("w_tm", [D, 2080])
    conv_w = din("conv_w", [128, 36, 4])
    conv_b = din("conv_b", [128, 36])
    hp = din("hp", [64, 64])
    normg = din("normg", [64, 2048])
    w_out = din("w_out", [2048, D])
    r_w = din("r_w", [D, NE])
    r_b = din("r_b", [128, NE])
    wgu = din("wgu", [NE, D, 2048])
    bgu = din("bgu", [128, NE, 16])
    wdn = din("wdn", [NE, D, D])
    bdn = din("bdn", [NE, D])
    st_tail = din("st_tail", [4608, 3])
    st_S = din("st_S", [128, 8, 128])
    st_h = din("st_h", [128, 2, 512])
    c32d = din("c32", [128, 512])
    c16d = din("c16", [128, 256], BF16)

    x_out = dout("x_out", [NSEG * T, D])
    so_tail = nc.dram_tensor("so_tail", [4608, 3], F32)
    so_S = nc.dram_tensor("so_S", [128, 8, 128], F32)
    so_h = nc.dram_tensor("so_h", [128, 2, 512], F32)
    if dbg:
        d_x1 = dout("d_x1", [T, D])
        d_mix = dout("d_mix", [2048, T], BF16)

    projT = nc.dram_tensor("projT", [4608, T + 3], F32)
    ztok = nc.dram_tensor("ztok", [T, 2048], F32)
    x1d = nc.dram_tensor("x1d", [T, D], F32)
    mixd = nc.dram_tensor("mixd", [2048, T], BF16)

    with ExitStack() as top:
        S = Sched(nc)
        b = B(nc, S)
        Rproj = [S.res(f"proj{i}") for i in range(36)]
        Rz = S.res("ztok")
        Rx1 = S.res("x1d")
        Rmix = S.res("mixd")
        Rxo = S.res("x_out")
        Ryo = S.res("y_out")
        Rso = S.res("so")
        Rdbg = S.res("dbg")

        PD = [nc.alloc_psum_tensor(f"pd{i}", [128, 1024], F32) for i in range(4)]
        RB = [S.res(f"bank{i}") for i in range(8)]
        pst = {"b1": 0, "b2": 0}

        def pb1():
            i = pst["b1"] % 4
            pst["b1"] += 1
            return PD[i // 2][:, (i % 2) * 512:(i % 2) * 512 + 512], [RB[i]]

        def pb2():
            j = 2 + pst["b2"] % 2
            pst["b2"] += 1
            return PD[j][:, :], [RB[2 * j], RB[2 * j + 1]]

        c32, Rc = b.tile(top, [128, 512], F32, "c32")
        c16, Rc16 = b.tile(top, [128, 256], BF16, "c16")
        scr, Rscr = b.tile(top, [128, 8], F32, "scr")
        b.dma("sp", c32[:], c32d[:, :], [], [Rc])
        b.dma("sp", c16[:], c16d[:, :], [], [Rc16])
        ident = c32[:, 0:128]
        ones = c32[:, 128:256]
        tri = c32[0:64, 256:320]
        maskU = c32[0:64, 320:384]
        maskL = c32[0:64, 384:448]
        ident16 = c16[:, 0:128]
        ones16 = c16[:, 128:256]
        try:
            for seg in range(NSEG):
                segst = ExitStack()
                xo = seg * T
                b.k = 1000
                modb, Rmod = b.tile(segst, [128, 4, D], F32, "modb")
                G1, SH2, A2, G2 = range(4)
                SH1, A1 = 0, 1
                ph2 = segst.enter_context(ExitStack())
                small, Rsmall = b.tile(ph2, [64, NC, 32], F32, "small")
                hpt, Rhp = b.tile(ph2, [64, 64], F32, "hp")
                nea, Rnea = b.tile(ph2, [64, 32], F32, "nea")
                bt, Rbt = b.tile(ph2, [64, NC, 8], F32, "bt")
                gg, Rgg = b.tile(ph2, [64, NC, 8], F32, "gg")
                gam, Rgam = b.tile(ph2, [64, NC, 8], F32, "gam")
                gamL, RgamL = b.tile(ph2, [64, NC, 8], F32, "gamL")
                dtt, Rdt = b.tile(ph2, [64, NC, 16], F32, "dt")
                aa, Raa = b.tile(ph2, [64, NC, 16], F32, "aa")
                acum, Racum = b.tile(ph2, [64, NC, 16], F32, "acum")
                acumL, RacumL = b.tile(ph2, [64, NC, 16], F32, "acumL")
                phMA = ExitStack()
                moda, Rmoda = b.tile(phMA, [128, 2, D], F32, "moda")

                def barrier():
                    S.barrier(lambda e: e.memset(scr[:, 0:1], 0.0))

                with ExitStack() as ph:
                    cv, Rcv = b.tile(ph, [128, 8], F32, "cv")
                    sg, Rsg = b.tile(ph, [128, 8], F32, "sg")
                    ca, Rca = b.tile(ph, [128, 8], F32, "ca")
                    b.dma("sp", cv[:], cvec[seg], [], [Rcv])
                    b.act(sg[:], cv[:], AF.Sigmoid, [Rcv], [Rsg])
                    b.op("dve", "tensor_tensor", [Rcv, Rsg], [Rca], out=ca[:], in0=cv[:], in1=sg[:], op=ALU.mult)
                    awt = [b.tile(ph, [128, 8, 512], F32, "aw") for _ in range(2)]
                    abt, Rab = b.tile(ph, [1, 6144], F32, "ab")
                    b.dma("sp", abt[:], ada_b[:, :], [], [Rab])
                    mrow, Rmrow = b.tile(ph, [1, 512], F32, "mrow")
                    adv = ada_w.rearrange("(k p) n -> p k n", p=128)
                    for j in range(12):
                        aw, Raw = awt[j % 2]
                        b.dma("sp", aw[:], adv[:, :, j * 512:(j + 1) * 512], [], [Raw])
                        ps, rp = pb1()
                        for k in range(8):
                            b.mm(ps[0:1, :], ca[:, k:k + 1], aw[:, k, :], k == 0, k == 7, [Rca, Raw], rp)
                        b.op("dve", "tensor_tensor", rp + [Rab], [Rmrow], out=mrow[:], in0=ps[0:1, :],
                             in1=abt[:, j * 512:(j + 1) * 512], op=ALU.add)
                        ps2, rp2 = pb1()
                        b.mm(ps2, ones[0:1, :], mrow[:], True, True, [Rc, Rmrow], rp2)
                        if j < 4:
                            b.op("act", "copy", rp2, [Rmoda], out=moda[:, j // 2, (j % 2) * 512:(j % 2) * 512 + 512], in_=ps2)
                        else:
                            b.op("act", "copy", rp2, [Rmod], out=modb[:, j // 2 - 2, (j % 2) * 512:(j % 2) * 512 + 512], in_=ps2)
                    gt, Rgt = b.tile(ph, [128, D], F32, "gt")
                    for (mt, Rm, idx, gsrc) in ((moda, Rmoda, A1, n1g), (modb, Rmod, A2, n2g)):
                        b.dma("sp", gt[:], gsrc[:, :], [], [Rgt])
                        b.op("dve", "scalar_tensor_tensor", [Rm, Rgt], [Rm], out=mt[:, idx, :], in0=mt[:, idx, :],
                             scalar=1.0, in1=gt[:], op0=ALU.add, op1=ALU.mult)
                barrier()
                if stage == 0:
                    b.dma("sp", x_out[0:128, :], modb[:, 0, :], [Rmod], [Rxo], final=True)
                    raise _Early()

                if True:
                    with ExitStack() as ph:
                        hT, RhT = b.tile(ph, [128, 8, T], BF16, "hT")
                        pha = ExitStack()
                        xts = [b.tile(pha, [128, D], F32, "xt") for _ in range(2)]
                        junk, Rjunk = b.tile(pha, [128, D], F32, "junk")
                        hb, Rhb = b.tile(pha, [128, D], BF16, "hb")
                        st1, Rst1 = b.tile(pha, [128, 4], F32, "st1")
                        for i in range(NT):
                            xt, Rxt = xts[i % 2]
                            b.dma("sp", xt[:], x_in[xo + i * 128:xo + (i + 1) * 128, :], [], [Rxt])
                            b.act(junk[:], xt[:], AF.Square, [Rxt], [Rjunk, Rst1], accum_out=st1[:, 0:1])
                            b.act(st1[:, 1:2], st1[:, 0:1], AF.Ln, [Rst1], [Rst1], scale=1.0 / D, bias=EPS)
                            b.act(st1[:, 2:3], st1[:, 1:2], AF.Exp, [Rst1], [Rst1], scale=-0.5)
                            b.op("dve", "scalar_tensor_tensor", [Rxt, Rst1, Rmoda], [Rjunk], out=junk[:], in0=xt[:],
                                 scalar=st1[:, 2:3], in1=moda[:, A1, :], op0=ALU.mult, op1=ALU.mult)
                            b.op("pool", "tensor_tensor", [Rjunk, Rmoda], [Rhb], out=hb[:], in0=junk[:], in1=moda[:, SH1, :], op=ALU.add)
                            ps, rp = pb1()
                            psb = ps.bitcast(BF16)
                            for k in range(8):
                                b.tr(psb[:, k * 128:(k + 1) * 128], hb[:, k * 128:(k + 1) * 128], ident16, [Rhb, Rc16], rp)
                            b.op("act", "copy", rp, [RhT], out=hT[:, :, i * 128:(i + 1) * 128],
                                 in_=psb.rearrange("p (k t) -> p k t", k=8))
                        pha.close()
                        barrier()
                        wv = w_cm.rearrange("(k p) n -> p k n", p=128)
                        wst = [b.tile(ph, [128, 8, 128], F32, "wst") for _ in range(2)]
                        w16 = [b.tile(ph, [128, 8, 128], BF16, "w16") for _ in range(2)]
                        SW = min(T, 1024)
                        stg = [b.tile(ph, [128, SW], F32, "stg") for _ in range(2)]
                        nst = 0
                        for c in range(36):
                            ws, Rws = wst[c % 2]
                            wb, Rwb = w16[c % 2]
                            b.dma("sp", ws[:], wv[:, :, c * 128:(c + 1) * 128], [], [Rws])
                            b.op(b.alt(["dve", "pool"]), "tensor_copy", [Rws], [Rwb], out=wb[:], in_=ws[:])
                            for s0 in range(0, T, SW):
                                sg_, Rsg_ = stg[nst % 2]
                                nst += 1
                                for tb in range(SW // 512):
                                    ps, rp = pb1()
                                    t0 = s0 + tb * 512
                                    for k in range(8):
                                        b.mm(ps, wb[:, k, :], hT[:, k, t0:t0 + 512], k == 0, k == 7, [Rwb, RhT], rp)
                                    if tb % 2 == 0:
                                        b.op("act", "copy", rp, [Rsg_], out=sg_[:, tb * 512:(tb + 1) * 512], in_=ps)
                                    else:
                                        b.op("dve", "tensor_copy", rp, [Rsg_], out=sg_[:, tb * 512:(tb + 1) * 512], in_=ps)
                                b.dma("sp", projT[c * 128:(c + 1) * 128, 3 + s0:3 + s0 + SW], sg_[:], [Rsg_], [Rproj[c]],
                                      same_gen=(s0 > 0))
                        wtv = w_tm.rearrange("(k p) n -> p k n", p=128)
                        wt16, Rwt16 = b.tile(ph, [128, 8, 2080], BF16, "wt16")
                        zst = [b.tile(ph, [128, 2080], F32, "zst") for _ in range(2)]
                        for k in range(8):
                            zs, Rzs = zst[k % 2]
                            b.dma("sp", zs[:], wtv[:, k, :], [], [Rzs])
                            b.op(b.alt(["dve", "pool"]), "tensor_copy", [Rzs], [Rwt16], out=wt16[:, k, :], in_=zs[:])
                        for i in range(NT):
                            zs, Rzs = zst[i % 2]
                            for q in range(4):
                                ps, rp = pb1()
                                for k in range(8):
                                    b.mm(ps, hT[:, k, i * 128:(i + 1) * 128], wt16[:, k, q * 512:(q + 1) * 512], k == 0, k == 7,
                                         [RhT, Rwt16], rp)
                                b.act(zs[:, q * 512:(q + 1) * 512], ps, AF.Silu, rp, [Rzs])
                            b.dma("sp", ztok[i * 128:(i + 1) * 128, :], zs[:, 0:2048], [Rzs], [Rz], same_gen=(i > 0))
                        for c in range(NC):
                            ps, rp = pb1()
                            for k in range(8):
                                b.mm(ps[0:64, 0:32], hT[:, k, c * 64:(c + 1) * 64], wt16[:, k, 2048:2080], k == 0, k == 7,
                                     [RhT, Rwt16], rp)
                            b.op("dve", "tensor_copy", rp, [Rsmall], out=small[:, c, :], in_=ps[0:64, 0:32])
                    phMA.close()
                    barrier()
                    if stage == 1:
                        b.dma("sp", x_out[0:64, 0:32], small[:, 0, :], [Rsmall], [Rxo], final=True)
                        raise _Early()

                    ph = segst.enter_context(ExitStack())
                    phD = ph
                    b.dma("sp", hpt[:], hp[:, :], [], [Rhp])
                    b.act(nea[:, 0:8], hpt[:, 0:8], AF.Exp, [Rhp], [Rnea])
                    b.act(nea[:, 8:24], hpt[:, 16:32], AF.Exp, [Rhp], [Rnea])
                    b.op("dve", "tensor_scalar", [Rnea], [Rnea], out=nea[:, 0:24], in0=nea[:, 0:24], scalar1=-1.0, scalar2=None,
                         op0=ALU.mult)
                    b.act(bt[:], small[:, :, 0:8], AF.Sigmoid, [Rsmall], [Rbt])
                    b.op("dve", "tensor_tensor", [Rsmall, Rhp], [Rgg], out=gg[:], in0=small[:, :, 8:16],
                         in1=bc(hpt[:, 8:16], [64, NC, 8], 1), op=ALU.add)
                    b.act(gg[:], gg[:], AF.Exp, [Rgg], [Rgg])
                    b.act(gg[:], gg[:], AF.Ln, [Rgg], [Rgg], bias=1.0)
                    b.op("dve", "tensor_tensor", [Rgg, Rnea], [Rgg], out=gg[:], in0=gg[:], in1=bc(nea[:, 0:8], [64, NC, 8], 1),
                         op=ALU.mult)
                    b.op("dve", "tensor_tensor", [Rsmall, Rhp], [Rdt], out=dtt[:], in0=small[:, :, 16:32],
                         in1=bc(hpt[:, 32:48], [64, NC, 16], 1), op=ALU.add)
                    b.act(dtt[:], dtt[:], AF.Exp, [Rdt], [Rdt])
                    b.act(dtt[:], dtt[:], AF.Ln, [Rdt], [Rdt], bias=1.0)
                    b.op("dve", "tensor_tensor", [Rdt, Rnea], [Raa], out=aa[:], in0=dtt[:], in1=bc(nea[:, 8:24], [64, NC, 16], 1),
                         op=ALU.mult)
                    ggf = gg[:].rearrange("p c h -> p (c h)")
                    aaf = aa[:].rearrange("p c h -> p (c h)")
                    for (src, Rsrc, dst, Rdst, lhs, width) in (
                        (ggf, Rgg, gam, Rgam, tri, NC * 8), (ggf, Rgg, gamL, RgamL, ones[0:64, 0:64], NC * 8),
                        (aaf, Raa, acum, Racum, tri, NC * 16), (aaf, Raa, acumL, RacumL, ones[0:64, 0:64], NC * 16)):
                        dstf = dst[:].rearrange("p c h -> p (c h)")
                        for n0 in range(0, width, 512):
                            n1 = min(width, n0 + 512)
                            ps, rp = pb1()
                            b.mm(ps[0:64, 0:n1 - n0], lhs, src[:, n0:n1], True, True, [Rc, Rsrc], rp)
                            b.op("dve", "tensor_copy", rp, [Rdst], out=dstf[:, n0:n1], in_=ps[0:64, 0:n1 - n0])

                    if stage == 2:
                        b.dma("sp", x_out[0:64, 0:NC * 8], gam[:].rearrange("p c h -> p (c h)"), [Rgam], [Rxo], final=True)
                        raise _Early()
                    cw, Rcw = b.tile(ph, [128, 36, 4], F32, "cw")
                    cb, Rcb = b.tile(ph, [128, 36], F32, "cb")
                    b.dma("sp", cw[:], conv_w[:, :, :], [], [Rcw])
                    b.dma("sp", cb[:], conv_b[:, :], [], [Rcb])
                    ngt, Rng = b.tile(ph, [64, 2048], F32, "ng")
                    b.dma("sp", ngt[:], normg[:, :], [], [Rng])
                    for c in range(36):
                        b.dma("sp", projT[c * 128:(c + 1) * 128, 0:3], (st_tail if seg % 2 == 0 else so_tail)[c * 128:(c + 1) * 128, :],
                              ([] if seg % 2 == 0 else [Rso]), [Rproj[c]], same_gen=True)
                    Sst, RS = b.tile(ph, [128, 8, 128], F32, "S")
                    Sb, RSb = b.tile(ph, [128, 8, 128], BF16, "Sb")
                    Hst, RH = b.tile(ph, [128, 2, 512], F32, "H")
                    Hb, RHb = b.tile(ph, [128, 2, 512], BF16, "Hb")
                    b.dma("sp", Sst[:], (st_S if seg % 2 == 0 else so_S)[:, :, :], ([] if seg % 2 == 0 else [Rso]), [RS])
                    b.dma("sp", Hst[:], (st_h if seg % 2 == 0 else so_h)[:, :, :], ([] if seg % 2 == 0 else [Rso]), [RH])
                    b.op("act", "copy", [RS], [RSb], out=Sb[:], in_=Sst[:])
                    b.op("act", "copy", [RH], [RHb], out=Hb[:], in_=Hst[:])
                    if stage == 2.2:
                        raise _Early()
                    BW = 256
                    CPB = BW // 64
                    blk, Rblk = b.tile(ph, [128, 36, BW], BF16, "blk")
                    Rblkc = [S.res(f"blk{c}") for c in range(36)]
                    mixT, RmixT = b.tile(ph, [128, 16, BW], BF16, "mixT")
                    lds = [b.tile(ph, [128, BW + 3], F32, "ld") for _ in range(2)]
                    cvs = [b.tile(ph, [128, BW], F32, "cvt") for _ in range(2)]
                    sq16, Rsq16 = b.tile(ph, [128, BW], BF16, "sq16")
                    lnt, Rlnt = b.tile(ph, [128, BW], F32, "lnt")
                    egb, Regb = b.tile(ph, [64, CPB, 8], F32, "egb")
                    kds, Rkds = b.tile(ph, [64, CPB, 8], F32, "kds")
                    bteg, Rbteg = b.tile(ph, [64, CPB, 8], F32, "bteg")
                    nbt, Rnbt = b.tile(ph, [64, CPB, 8], F32, "nbt")
                    xds, Rxds = b.tile(ph, [64, CPB, 16], F32, "xds")
                    eab, Reab = b.tile(ph, [64, CPB, 16], F32, "eab")
                    Kc, RKc = b.tile(ph, [64, 8, 128], BF16, "Kc")
                    Kt, RKt = b.tile(ph, [64, 8, 128], BF16, "Kt")
                    Kd, RKd = b.tile(ph, [64, 8, 128], BF16, "Kd")
                    Vc, RVc = b.tile(ph, [64, 8, 128], BF16, "Vc")
                    bV, RbV = b.tile(ph, [64, 8, 128], BF16, "bV")
                    Xc, RXc = b.tile(ph, [64, 16, 64], BF16, "Xc")
                    Xdt, RXdt = b.tile(ph, [64, 16, 64], BF16, "Xdt")
                    Xde, RXde = b.tile(ph, [64, 16, 64], BF16, "Xde")
                    XsD, RXsD = b.tile(ph, [64, 16, 64], BF16, "XsD")
                    Bmc, RBmc = b.tile(ph, [64, 2, 128], BF16, "Bmc")
                    rhsG, RrhsG = b.tile(ph, [64, 8, 64], F32, "rhsG")
                    E1, RE1 = b.tile(ph, [64, 8, 64], F32, "E1")
                    Dt_, RDt = b.tile(ph, [64, 8, 64], F32, "Dt")
                    decS, RdecS = b.tile(ph, [64, 8, 64], F32, "decS")
                    decT, RdecT = b.tile(ph, [64, 8, 64], F32, "decT")
                    egr, Regr = b.tile(ph, [128, 8, 64], F32, "egr")
                    qdT, RqdT = b.tile(ph, [128, 8, 64], BF16, "qdT")
                    Mk = [b.tile(ph, [64, 8, 64], F32, "Mk") for _ in range(2)]
                    MkT = [b.tile(ph, [64, 8, 64], F32, "MkT") for _ in range(2)]
                    Qk = [b.tile(ph, [64, 8, 64], F32, "Qk") for _ in range(2)]
                    Qb, RQb = b.tile(ph, [64, 8, 64], BF16, "Qb")
                    qkm, Rqkm = b.tile(ph, [64, 8, 64], BF16, "qkm")
                    ut, Rut = b.tile(ph, [64, 8, 128], F32, "ut")
                    wT, RwT = b.tile(ph, [128, 8, 64], BF16, "wT")
                    vnb, Rvnb = b.tile(ph, [64, 8, 128], BF16, "vnb")
                    rhsA, RrhsA = b.tile(ph, [64, 16, 64], F32, "rhsA")
                    E1s, RE1s = b.tile(ph, [64, 16, 64], F32, "E1s")
                    dTs, RdTs = E1s, RE1s
                    MhT, RMhT = b.tile(ph, [64, 16, 64], BF16, "MhT")
                    cds, Rcds = b.tile(ph, [128, 16], F32, "cds")
                    cbs, Rcbs = b.tile(ph, [64, 128], F32, "cbs")
                    y1, Ry1 = rhsA, RrhsA
                    zt, Rzt = b.tile(ph, [64, 2048], F32, "zt")
                    ot, Rot = b.tile(ph, [64, 2048], F32, "ot")
                    o2h, Ro2 = b.tile(ph, [64, 1024], F32, "o2")
                    mo, Rmo = b.tile(ph, [64, 2048], BF16, "mo")
                    ssn, Rssn = b.tile(ph, [64, 16], F32, "ssn")

                    QOFF, KOFF, VOFF, XOFF, BOFF, COFF = 0, 8, 16, 24, 32, 34

                    for blkI in range(T // BW):
                        t0 = blkI * BW
                        for c in range(36):
                            ld, Rld = lds[c % 2]
                            cvt, Rcvt = cvs[c % 2]
                            b.dma("sp", ld[:], projT[c * 128:(c + 1) * 128, t0:t0 + BW + 3], [Rproj[c]], [Rld])
                            e1 = "dve"
                            b.op(e1, "tensor_scalar", [Rld, Rcw, Rcb], [Rcvt], out=cvt[:], in0=ld[:, 0:BW],
                                 scalar1=cw[:, c, 0:1], scalar2=cb[:, c:c + 1], op0=ALU.mult, op1=ALU.add)
                            for j in range(1, 4):
                                b.op(e1, "scalar_tensor_tensor", [Rld, Rcw, Rcvt], [Rcvt], out=cvt[:], in0=ld[:, j:j + BW],
                                     scalar=cw[:, c, j:j + 1], in1=cvt[:], op0=ALU.mult, op1=ALU.add)
                            if c < 16:
                                b.act(cvt[:], cvt[:], AF.Silu, [Rcvt], [Rcvt])
                                b.op("pool", "tensor_tensor", [Rcvt], [Rsq16], out=sq16[:], in0=cvt[:], in1=cvt[:], op=ALU.mult)
                                ps, rp = pb1()
                                b.mm(ps[:, 0:BW], ones16, sq16[:], True, True, [Rc16, Rsq16], rp)
                                b.act(lnt[:], ps[:, 0:BW], AF.Ln, rp, [Rlnt], bias=EPS)
                                b.act(lnt[:], lnt[:], AF.Exp, [Rlnt], [Rlnt], scale=-0.5,
                                      bias=(float(np.log(128.0 ** -0.5)) if c < 8 else 0.0))
                                b.op("dve", "tensor_tensor", [Rcvt, Rlnt], [Rblkc[c]], out=blk[:, c, :], in0=cvt[:], in1=lnt[:],
                                     op=ALU.mult)
                            else:
                                b.act(blk[:, c, :], cvt[:], AF.Silu, [Rcvt], [Rblkc[c]])
                        cs = slice(blkI * CPB, blkI * CPB + CPB)
                        b.act(egb[:], gam[:, cs, :], AF.Exp, [Rgam], [Regb])
                        b.op("dve", "tensor_tensor", [RgamL, Rgam], [Rkds], out=kds[:], in0=gamL[:, cs, :], in1=gam[:, cs, :],
                             op=ALU.subtract)
                        b.act(kds[:], kds[:], AF.Exp, [Rkds], [Rkds])
                        b.op("dve", "tensor_tensor", [Rbt, Regb], [Rbteg], out=bteg[:], in0=bt[:, cs, :], in1=egb[:], op=ALU.mult)
                        b.op("dve", "tensor_scalar", [Rbt], [Rnbt], out=nbt[:], in0=bt[:, cs, :], scalar1=-1.0, scalar2=None,
                             op0=ALU.mult)
                        b.op("dve", "tensor_tensor", [RacumL, Racum], [Rxds], out=xds[:], in0=acumL[:, cs, :], in1=acum[:, cs, :],
                             op=ALU.subtract)
                        b.act(xds[:], xds[:], AF.Exp, [Rxds], [Rxds])
                        b.op("dve", "tensor_tensor", [Rxds, Rdt], [Rxds], out=xds[:], in0=xds[:], in1=dtt[:, cs, :], op=ALU.mult)
                        b.act(eab[:], acum[:, cs, :], AF.Exp, [Racum], [Reab])
                        if stage == 2.4:
                            raise _Early()

                        for ci in range(CPB):
                            cg = blkI * CPB + ci
                            co = ci * 64
                            def tposes(c0, n, width):
                                ps, rp = pb1()
                                psb = ps.bitcast(BF16)
                                for h in range(n):
                                    b.tr(psb[0:64, h * 128:(h + 1) * 128], blk[:, c0 + h, co:co + 64], ident16,
                                         [Rblkc[c0 + h], Rc16], rp)
                                return psb, rp
                            psb, rp = tposes(KOFF, 8, 1024)
                            b.op("act", "copy", rp, [RKc], out=Kc[:], in_=psb[0:64, :].rearrange("p (h d) -> p h d", h=8))
                            psb, rp = tposes(VOFF, 8, 1024)
                            b.op("act", "copy", rp, [RVc], out=Vc[:], in_=psb[0:64, :].rearrange("p (h d) -> p h d", h=8))
                            psb, rp = tposes(XOFF, 8, 1024)
                            b.op("act", "copy", rp, [RXc], out=Xc[:], in_=psb[0:64, :].rearrange("p (h d) -> p h d", h=16))
                            psb, rp = tposes(BOFF, 2, 256)
                            b.op("act", "copy", rp, [RBmc], out=Bmc[:], in_=psb[0:64, 0:256].rearrange("p (h d) -> p h d", h=2))
                            b.op("pool", "tensor_tensor", [RKc, Rbteg], [RKt], out=Kt[:], in0=Kc[:],
                                 in1=bc(bteg[:, ci, :], [64, 8, 128], 2), op=ALU.mult)
                            b.op("pool", "tensor_tensor", [RKc, Rkds], [RKd], out=Kd[:], in0=Kc[:],
                                 in1=bc(kds[:, ci, :], [64, 8, 128], 2), op=ALU.mult)
                            b.op("pool", "tensor_tensor", [RVc, Rbt], [RbV], out=bV[:], in0=Vc[:],
                                 in1=bc(bt[:, cg, :], [64, 8, 128], 2), op=ALU.mult)
                            b.op("pool", "tensor_tensor", [RXc, Rdt], [RXdt], out=Xdt[:], in0=Xc[:],
                                 in1=bc(dtt[:, cg, :], [64, 16, 64], 2), op=ALU.mult)
                            b.op("pool", "tensor_tensor", [RXc, Rxds], [RXde], out=Xde[:], in0=Xc[:],
                                 in1=bc(xds[:, ci, :], [64, 16, 64], 2), op=ALU.mult)
                            b.op("pool", "tensor_tensor", [RXc, Rhp], [RXsD], out=XsD[:], in0=Xc[:],
                                 in1=bc(hpt[:, 48:64], [64, 16, 64], 2), op=ALU.mult)
                            if stage == 2.5:
                                raise _Early()

                            b.op("dve", "tensor_tensor", [Rgg, Rc], [RrhsG], out=rhsG[:], in0=bc(gg[:, cg, :], [64, 8, 64], 2),
                                 in1=bc(tri, [64, 8, 64], 1), op=ALU.mult)
                            psg, rpg = pb1()
                            b.mm(psg, ones[0:64, :], rhsG[:].rearrange("p h l -> p (h l)"), True, True, [Rc, RrhsG], rpg)
                            if stage == 2.51:
                                raise _Early()
                            b.op("act", "copy", rpg, [Regr], out=egr[:].rearrange("p h l -> p (h l)"), in_=psg)
                            b.op("dve", "tensor_tensor", [Regr, Rgam], [RE1], out=E1[:], in0=egr[0:64],
                                 in1=bc(gam[:, cg, :], [64, 8, 64], 2), op=ALU.subtract)
                            b.act(egr[:], egr[:], AF.Exp, [Regr], [Regr])
                            if stage == 2.52:
                                raise _Early()
                            b.op("dve", "scalar_tensor_tensor", [RE1, Rc], [RDt], out=Dt_[:], in0=E1[:], scalar=0.0,
                                 in1=bc(maskU, [64, 8, 64], 1), op0=ALU.max, op1=ALU.add)
                            b.act(decS[:], Dt_[:], AF.Exp, [RDt], [RdecS], scale=-1.0)
                            b.op("dve", "scalar_tensor_tensor", [RE1, Rc], [RDt], out=Dt_[:], in0=E1[:], scalar=0.0,
                                 in1=bc(maskL, [64, 8, 64], 1), op0=ALU.min, op1=ALU.add)
                            b.act(decT[:], Dt_[:], AF.Exp, [RDt], [RdecT])
                            if stage == 2.53:
                                raise _Early()
                            b.op("dve", "tensor_tensor", [Rblkc[QOFF + h] for h in range(8)] + [Regr], [RqdT], out=qdT[:],
                                 in0=blk[:, QOFF:QOFF + 8, co:co + 64], in1=egr[:], op=ALU.mult)
                            if stage == 2.55:
                                raise _Early()
                            pkk, rkk = pb1()
                            pqk, rqk = pb1()
                            for h in range(8):
                                kTh = blk[:, KOFF + h, co:co + 64]
                                b.mm(pkk[0:64, h * 64:(h + 1) * 64], kTh, kTh, True, True, [Rblkc[KOFF + h]], rkk)
                                b.mm(pqk[0:64, h * 64:(h + 1) * 64], kTh, blk[:, QOFF + h, co:co + 64], True, True,
                                     [Rblkc[KOFF + h], Rblkc[QOFF + h]], rqk)
                            pkk3 = pkk[0:64, :].rearrange("p (h l) -> p h l", h=8)
                            pqk3 = pqk[0:64, :].rearrange("p (h l) -> p h l", h=8)
                            b.op("dve", "tensor_tensor", rqk + [RdecT], [Rqkm], out=qkm[:], in0=pqk3, in1=decT[:], op=ALU.mult)
                            M0, RM0 = Mk[0]
                            M0T, RM0T = MkT[0]
                            b.op("act", "copy", rkk, [RM0], out=M0[:].rearrange("p h l -> p (h l)"), in_=pkk[0:64, :])
                            b.op("dve", "tensor_tensor", [RM0, Rnbt], [RM0], out=M0[:], in0=M0[:],
                                 in1=bc(nbt[:, ci, :], [64, 8, 64], 2), op=ALU.mult)
                            b.op("pool", "tensor_tensor", [RM0, RdecS], [RM0], out=M0[:], in0=M0[:], in1=decS[:], op=ALU.mult)
                            if stage == 2.57:
                                raise _Early()
                            pt, rpt = pb1()
                            for h in range(8):
                                b.tr(pt[0:64, h * 64:(h + 1) * 64], M0[:, h, :], ident[0:64, 0:64], [RM0, Rc], rpt)
                            b.op("act", "copy", rpt, [RM0T], out=M0T[:], in_=pt[0:64, :].rearrange("p (h l) -> p h l", h=8))
                            Q0, RQ0 = Qk[0]
                            b.op("dve", "tensor_tensor", [RM0T, Rc], [RQ0], out=Q0[:], in0=M0T[:],
                                 in1=bc(ident[0:64, 0:64], [64, 8, 64], 1), op=ALU.add)
                            if stage == 2.58:
                                raise _Early()
                            cur = 0
                            for lev in range(5):
                                Mc, RMc = Mk[cur]
                                McT, RMcT = MkT[cur]
                                Mn, RMn = Mk[1 - cur]
                                MnT, RMnT = MkT[1 - cur]
                                Qc, RQc = Qk[cur]
                                Qn, RQn = Qk[1 - cur]
                                pa, rpa = pb1()
                                for h in range(8):
                                    b.mm(pa[0:64, h * 64:(h + 1) * 64], McT[:, h, :], Mc[:, h, :], True, True, [RMc, RMcT], rpa)
                                if lev < 4:
                                    pbt, rpb = pb1()
                                    for h in range(8):
                                        b.mm(pbt[0:64, h * 64:(h + 1) * 64], Mc[:, h, :], McT[:, h, :], True, True, [RMc, RMcT], rpb)
                                b.op("act", "copy", rpa, [RMn], out=Mn[:], in_=pa[0:64, :].rearrange("p (h l) -> p h l", h=8))
                                if lev < 4:
                                    b.op("dve", "tensor_copy", rpb, [RMnT], out=MnT[:],
                                         in_=pbt[0:64, :].rearrange("p (h l) -> p h l", h=8))
                                pc, rpc = pb1()
                                for h in range(8):
                                    b.mm(pc[0:64, h * 64:(h + 1) * 64], Mn[:, h, :], Qc[:, h, :], True, True, [RMn, RQc], rpc)
                                b.op("dve", "tensor_tensor", rpc + [RQc], [RQn], out=Qn[:],
                                     in0=pc[0:64, :].rearrange("p (h l) -> p h l", h=8), in1=Qc[:], op=ALU.add)
                                cur = 1 - cur
                            Qf, RQf = Qk[cur]
                            b.op("act", "copy", [RQf], [RQb], out=Qb[:], in_=Qf[:])
                            if stage == 2.6:
                                raise _Early()
                            pu, rpu = pb2()
                            pw, rpw = pb1()
                            for h in range(8):
                                b.mm(pu[0:64, h * 128:(h + 1) * 128], Qb[:, h, :], bV[:, h, :], True, True, [RQb, RbV], rpu)
                                b.mm(pw[:, h * 64:(h + 1) * 64], Kt[:, h, :], Qb[:, h, :], True, True, [RKt, RQb], rpw)
                            b.op("act", "copy", rpu, [Rut], out=ut[:], in_=pu[0:64, :].rearrange("p (h d) -> p h d", h=8))
                            b.op("dve", "tensor_copy", rpw, [RwT], out=wT[:], in_=pw.rearrange("p (h l) -> p h l", h=8))
                            pv, rpv = pb2()
                            for h in range(8):
                                b.mm(pv[0:64, h * 128:(h + 1) * 128], wT[:, h, :], Sb[:, h, :], True, True, [RwT, RSb], rpv)
                            b.op("dve", "tensor_tensor", [Rut] + rpv, [Rvnb], out=vnb[:], in0=ut[:],
                                 in1=pv[0:64, :].rearrange("p (h d) -> p h d", h=8), op=ALU.subtract)
                            po, rpo = pb2()
                            for h in range(8):
                                b.mm(po[0:64, h * 128:(h + 1) * 128], qdT[:, h, :], Sb[:, h, :], True, False, [RqdT, RSb], rpo)
                                b.mm(po[0:64, h * 128:(h + 1) * 128], qkm[:, h, :], vnb[:, h, :], False, True, [Rqkm, Rvnb], rpo)
                            pds, rpds = pb2()
                            for h in range(8):
                                b.mm(pds[:, h * 128:(h + 1) * 128], Kd[:, h, :], vnb[:, h, :], True, True, [RKd, Rvnb], rpds)
                            b.op("dve", "tensor_tensor", [RS, Regr], [RS], out=Sst[:], in0=Sst[:],
                                 in1=egr[:, :, 63:64].to_broadcast([128, 8, 128]), op=ALU.mult)
                            b.op("dve", "tensor_tensor", [RS] + rpds, [RS], out=Sst[:], in0=Sst[:],
                                 in1=pds.rearrange("p (h d) -> p h d", h=8), op=ALU.add)
                            b.op("act", "copy", [RS], [RSb], out=Sb[:], in_=Sst[:])
                            b.dma("sp", zt[:], ztok[cg * 64:(cg + 1) * 64, :], [Rz], [Rzt])
                            po3 = po[0:64, :].rearrange("p (h d) -> p h d", h=8)
                            b.op("act", "copy", rpo, [Rot], out=ot[:, 0:1024], in_=po[0:64, :])
                            b.op("pool", "tensor_tensor", [Rot], [Ro2], out=o2h[:, :], in0=ot[:, 0:1024], in1=ot[:, 0:1024],
                                 op=ALU.mult)
                            b.op("dve", "tensor_reduce", [Ro2], [Rssn], out=ssn[:, 0:8],
                                 in_=o2h[:, :].rearrange("p (h d) -> p h d", h=8), axis=AX.X, op=ALU.add)
                            b.act(ssn[:, 0:8], ssn[:, 0:8], AF.Ln, [Rssn], [Rssn], scale=1.0 / 128, bias=EPS)
                            b.act(ssn[:, 0:8], ssn[:, 0:8], AF.Exp, [Rssn], [Rssn], scale=-0.5)
                            b.op("dve", "tensor_tensor", [Rot, Rssn], [Ro2], out=o2h[:, :].rearrange("p (h d) -> p h d", h=8),
                                 in0=ot[:, 0:1024].rearrange("p (h d) -> p h d", h=8), in1=bc(ssn[:, 0:8], [64, 8, 128], 2),
                                 op=ALU.mult)
                            b.op("pool", "tensor_tensor", [Ro2, Rng], [Ro2], out=o2h[:, :], in0=o2h[:, :],
                                 in1=ngt[:, 0:1024], op=ALU.mult)
                            b.op("pool", "tensor_tensor", [Ro2, Rzt], [Rmo], out=mo[:, 0:1024], in0=o2h[:, :],
                                 in1=zt[:, 0:1024], op=ALU.mult)
                            if stage == 2.7:
                                raise _Early()

                            b.op("dve", "tensor_tensor", [Raa, Rc], [RrhsA], out=rhsA[:], in0=bc(aa[:, cg, :], [64, 16, 64], 2),
                                 in1=bc(tri, [64, 16, 64], 1), op=ALU.mult)
                            pa2, rpa2 = pb2()
                            rA = rhsA[:].rearrange("p h l -> p (h l)")
                            b.mm(pa2[:, 0:512], ones[0:64, :], rA[:, 0:512], True, True, [Rc, RrhsA], rpa2)
                            b.mm(pa2[:, 512:1024], ones[0:64, :], rA[:, 512:1024], True, True, [Rc, RrhsA], rpa2)
                            pa23 = pa2.rearrange("p (h l) -> p h l", h=16)
                            b.op("act", "copy", rpa2, [RE1s], out=E1s[:].rearrange("p h l -> p (h l)"), in_=pa2[0:64, :])
                            for hh in range(16):
                                b.act(cds[:, hh:hh + 1], pa2[:, hh * 64 + 63:hh * 64 + 64], AF.Exp, rpa2, [Rcds])
                            b.op("dve", "tensor_tensor", [RE1s, Racum], [RE1s], out=E1s[:], in0=E1s[:],
                                 in1=bc(acum[:, cg, :], [64, 16, 64], 2), op=ALU.subtract)
                            b.op("dve", "scalar_tensor_tensor", [RE1s, Rc], [RE1s], out=E1s[:], in0=E1s[:], scalar=0.0,
                                 in1=bc(maskL, [64, 16, 64], 1), op0=ALU.min, op1=ALU.add)
                            b.act(dTs[:], E1s[:], AF.Exp, [RE1s], [RdTs])
                            pcb, rcb = pb1()
                            for g in range(2):
                                b.mm(pcb[0:64, g * 64:(g + 1) * 64], blk[:, BOFF + g, co:co + 64], blk[:, COFF + g, co:co + 64],
                                     True, True, [Rblkc[BOFF + g], Rblkc[COFF + g]], rcb)
                            b.op("act", "copy", rcb, [Rcbs], out=cbs[:], in_=pcb[0:64, 0:128])
                            for g in range(2):
                                b.op("dve", "tensor_tensor", [RdTs, Rcbs], [RMhT], out=MhT[:, g * 8:(g + 1) * 8, :],
                                     in0=dTs[:, g * 8:(g + 1) * 8, :], in1=bc(cbs[:, g * 64:(g + 1) * 64], [64, 8, 64], 1),
                                     op=ALU.mult)
                            py, rpy = pb2()
                            for h in range(16):
                                b.mm(py[0:64, h * 64:(h + 1) * 64], MhT[:, h, :], Xdt[:, h, :], True, False, [RMhT, RXdt], rpy)
                                b.mm(py[0:64, h * 64:(h + 1) * 64], ident16[0:64, 0:64], XsD[:, h, :], False, True,
                                     [Rc16, RXsD], rpy)
                            pyo, rpyo = pb2()
                            for g in range(2):
                                b.mm(pyo[0:64, g * 512:(g + 1) * 512], blk[:, COFF + g, co:co + 64], Hb[:, g, :], True, True,
                                     [Rblkc[COFF + g], RHb], rpyo)
                            b.op("act", "copy", rpyo, [Ry1], out=y1[:].rearrange("p h d -> p (h d)"), in_=pyo[0:64, :])
                            b.op("dve", "tensor_tensor", [Ry1, Reab], [Ry1], out=y1[:], in0=y1[:],
                                 in1=bc(eab[:, ci, :], [64, 16, 64], 2), op=ALU.mult)
                            b.op("dve", "tensor_tensor", rpy + [Ry1], [Rot], out=ot[:, 1024:2048], in0=py[0:64, :],
                                 in1=y1[:].rearrange("p h d -> p (h d)"), op=ALU.add)
                            pdh, rpdh = pb2()
                            for g in range(2):
                                b.mm(pdh[:, g * 512:(g + 1) * 512], Bmc[:, g, :],
                                     Xde[:, g * 8:(g + 1) * 8, :].rearrange("p h d -> p (h d)"), True, True, [RBmc, RXde], rpdh)
                            b.op("dve", "tensor_tensor", [RH, Rcds], [RH], out=Hst[:].rearrange("p g (e d) -> p (g e) d", e=8),
                                 in0=Hst[:].rearrange("p g (e d) -> p (g e) d", e=8), in1=bc(cds[:], [128, 16, 64], 2), op=ALU.mult)
                            b.op("dve", "tensor_tensor", [RH] + rpdh, [RH], out=Hst[:], in0=Hst[:],
                                 in1=pdh.rearrange("p (g n) -> p g n", g=2), op=ALU.add)
                            b.op("act", "copy", [RH], [RHb], out=Hb[:], in_=Hst[:])
                            b.op("pool", "tensor_tensor", [Rot, Rzt], [Rot], out=ot[:, 1024:2048], in0=ot[:, 1024:2048],
                                 in1=zt[:, 1024:2048], op=ALU.mult)
                            b.op("pool", "tensor_tensor", [Rot], [Ro2], out=o2h[:, :], in0=ot[:, 1024:2048],
                                 in1=ot[:, 1024:2048], op=ALU.mult)
                            b.op("dve", "tensor_reduce", [Ro2], [Rssn], out=ssn[:, 8:10],
                                 in_=o2h[:, :].rearrange("p (g d) -> p g d", g=2), axis=AX.X, op=ALU.add)
                            b.act(ssn[:, 8:10], ssn[:, 8:10], AF.Ln, [Rssn], [Rssn], scale=1.0 / 512, bias=EPS)
                            b.act(ssn[:, 8:10], ssn[:, 8:10], AF.Exp, [Rssn], [Rssn], scale=-0.5)
                            b.op("dve", "tensor_tensor", [Rot, Rssn], [Ro2], out=o2h[:, :].rearrange("p (g d) -> p g d", g=2),
                                 in0=ot[:, 1024:2048].rearrange("p (g d) -> p g d", g=2), in1=bc(ssn[:, 8:10], [64, 2, 512], 2),
                                 op=ALU.mult)
                            b.op("pool", "tensor_tensor", [Ro2, Rng], [Rmo], out=mo[:, 1024:2048], in0=o2h[:, :],
                                 in1=ngt[:, 1024:2048], op=ALU.mult)
                            if stage == 2.8:
                                raise _Early()
                            pm, rpm = pb1()
                            pmb = pm.bitcast(BF16)
                            for j in range(16):
                                b.tr(pmb[:, j * 64:(j + 1) * 64], mo[:, j * 128:(j + 1) * 128], ident16[0:64, 0:64], [Rmo, Rc16], rpm)
                            b.op("act", "copy", rpm, [RmixT], out=mixT[:, :, co:co + 64], in_=pmb.rearrange("p (j l) -> p j l", j=16))
                            if stage == 2.9:
                                raise _Early()
                            if stage == 2.92 and ci == 1:
                                raise _Early()
                        b.dma("sp", mixd.rearrange("(j p) t -> p j t", p=128)[:, :, t0:t0 + BW], mixT[:], [RmixT], [Rmix],
                              same_gen=(blkI > 0))
                        if stage == 2.95:
                            raise _Early()
                    if stage == 2.97:
                        raise _Early()
                    for c in range(36):
                        b.dma("sp", so_tail[c * 128:(c + 1) * 128, :], projT[c * 128:(c + 1) * 128, T:T + 3], [Rproj[c]], [Rso],
                              same_gen=(c > 0))
                    b.dma("sp", so_S[:, :, :], Sst[:], [RS], [Rso], same_gen=True)
                    b.dma("sp", so_h[:, :, :], Hst[:], [RH], [Rso], same_gen=True)
                    if dbg:
                        b.dma("sp", d_mix[:, :], mixd[:, :], [Rmix], [Rdbg], final=True)
                if stage == 3:
                    raise _Early()
                phD.close()
                ph2.close()
                barrier()

                phE = ExitStack()
                ph = phE
                if True:
                    QT = min(T, 512)
                    NQ = T // QT
                    NTQ = QT // 128
                    wo16, Rwo = b.tile(ph, [128, 16, D], BF16, "wo16")
                    tmp, Rtmp = b.tile(ph, [128, D], F32, "tmp")
                    h2f, Rh2f = b.tile(ph, [128, D], F32, "h2f")
                    wov = w_out.rearrange("(k p) n -> p k n", p=128)
                    for k in range(16):
                        zs, Rzs = (tmp, Rtmp) if k % 2 == 0 else (h2f, Rh2f)
                        b.dma("sp", zs[:], wov[:, k, :], [], [Rzs])
                        b.op(b.alt(["dve", "pool"]), "tensor_copy", [Rzs], [Rwo], out=wo16[:, k, :], in_=zs[:])
                    rw, Rrw = b.tile(ph, [128, 8, NE], F32, "rw")
                    rbt, Rrb = b.tile(ph, [128, NE], F32, "rb")
                    b.dma("sp", rw[:], r_w.rearrange("(k p) n -> p k n", p=128), [], [Rrw])
                    rwhi, Rrwh = b.tile(ph, [128, 8, NE], BF16, "rwhi")
                    rwlo, _ = b.tile(ph, [128, 8, NE], BF16, "rwlo")
                    b.op("act", "copy", [Rrw], [Rrwh], out=rwhi[:], in_=rw[:])
                    b.op("dve", "tensor_tensor", [Rrw, Rrwh], [Rrwh], out=rwlo[:], in0=rw[:], in1=rwhi[:], op=ALU.subtract)
                    b.dma("sp", rbt[:], r_b[:, :], [], [Rrb])
                    bgt, Rbg = b.tile(ph, [128, NE, 16], F32, "bg")
                    b.dma("sp", bgt[:], bgu[:, :, :], [], [Rbg])
                    fgt, Rfg = b.tile(ph, [128, D], F32, "fgt")
                    b.dma("sp", fgt[:], fgd[:, :], [], [Rfg])
                    fint, Rfin = b.tile(ph, [128, 1], F32, "fin")
                    b.dma("sp", fint[:], find[:, :], [], [Rfin])
                    G, RG = b.tile(ph, [128, NTQ, NE], F32, "G")
                    h2T, Rh2T = b.tile(ph, [128, 8, QT], BF16, "h2T")
                    acc, Racc = b.tile(ph, [128, NTQ, D], F32, "acc")
                    mx, Rmx = b.tile(ph, [128, 16, 128], BF16, "mx")
                    mxf = mx[:].rearrange("p j t -> p (j t)")
                    hhi, Rhhi = mxf[:, 0:1024], Rmx
                    hlo, Rhlo = mxf[:, 1024:2048], Rmx
                    xt, Rxt = b.tile(ph, [128, D], F32, "xt2")
                    x1t, Rx1t = b.tile(ph, [128, D], F32, "x1t")
                    hloT, RhloT = b.tile(ph, [128, 8, 128], BF16, "hloT")
                    st2, Rst2 = b.tile(ph, [128, 8], F32, "st2")
                    lg, Rlg = b.tile(ph, [128, NE], F32, "lg")
                    m8, Rm8 = b.tile(ph, [128, 8], F32, "m8")
                    msk, Rmsk = b.tile(ph, [128, NE], F32, "msk")
                    ex, Rex = b.tile(ph, [128, NE], F32, "ex")
                    wg16 = [b.tile(ph, [128, 8, 2048], BF16, "wg16") for _ in range(2)]
                    wd16 = [b.tile(ph, [128, 8, D], BF16, "wd16")] * 2
                    bdt = [b.tile(ph, [1, D], F32, "bdt")] * 2
                    AT, RAT = b.tile(ph, [128, 8, 512], BF16, "AT")
                    bd16, Rbd16 = b.tile(ph, [1, D], BF16, "bd16")
                    gsb, Rgsb = b.tile(ph, [128, 512], F32, "gsb")
                    sgm, Rsgm = b.tile(ph, [128, 512], F32, "sgm")
                    usb, Rusb = b.tile(ph, [128, 512], F32, "usb")
                    mixv = mixd.rearrange("(j p) t -> p j t", p=128)
                    nld = 0
                    for q in range(NQ):
                        if stage == 4.01:
                            raise _Early()
                        for tt in range(NTQ):
                            tg = q * NTQ + tt
                            b.dma("sp", mx[:], mixv[:, :, tg * 128:(tg + 1) * 128], [Rmix], [Rmx])
                            b.dma("sp", xt[:], x_in[xo + tg * 128:xo + (tg + 1) * 128, :], [], [Rxt])
                            po, rpo = pb2()
                            for half in range(2):
                                for k in range(16):
                                    b.mm(po[:, half * 512:(half + 1) * 512], mx[:, k, :], wo16[:, k, half * 512:(half + 1) * 512],
                                         k == 0, k == 15, [Rmx, Rwo], rpo)
                            b.op("dve", "tensor_tensor", rpo + [Rmod], [Rtmp], out=tmp[:], in0=po, in1=modb[:, G1, :], op=ALU.mult)
                            b.op("pool", "tensor_tensor", [Rtmp, Rxt], [Rx1t], out=x1t[:], in0=tmp[:], in1=xt[:], op=ALU.add)
                            b.dma("sp", x1d[tg * 128:(tg + 1) * 128, :], x1t[:], [Rx1t], [Rx1], same_gen=(tg > 0))
                            if stage == 4.02:
                                raise _Early()
                            b.act(tmp[:], x1t[:], AF.Square, [Rx1t], [Rtmp, Rst2], accum_out=st2[:, 0:1])
                            b.act(st2[:, 1:2], st2[:, 0:1], AF.Ln, [Rst2], [Rst2], scale=1.0 / D, bias=EPS)
                            b.act(st2[:, 2:3], st2[:, 1:2], AF.Exp, [Rst2], [Rst2], scale=-0.5)
                            b.op("dve", "scalar_tensor_tensor", [Rx1t, Rst2, Rmod], [Rtmp], out=tmp[:], in0=x1t[:],
                                 scalar=st2[:, 2:3], in1=modb[:, A2, :], op0=ALU.mult, op1=ALU.mult)
                            b.op("pool", "tensor_tensor", [Rtmp, Rmod], [Rh2f], out=h2f[:], in0=tmp[:], in1=modb[:, SH2, :], op=ALU.add)
                            b.op("act", "copy", [Rh2f], [Rhhi], out=hhi, in_=h2f[:])
                            b.op("dve", "tensor_tensor", [Rh2f, Rhhi], [Rhlo], out=hlo, in0=h2f[:], in1=hhi, op=ALU.subtract)
                            pth, rpth = pb1()
                            pthb = pth.bitcast(BF16)
                            for k in range(8):
                                b.tr(pthb[:, k * 128:(k + 1) * 128], hhi[:, k * 128:(k + 1) * 128], ident16, [Rhhi, Rc16], rpth)
                            b.op("dve", "tensor_copy", rpth, [Rh2T], out=h2T[:, :, tt * 128:(tt + 1) * 128], in_=pthb.rearrange("p (k t) -> p k t", k=8))
                            ptl, rptl = pb1()
                            ptlb = ptl.bitcast(BF16)
                            for k in range(8):
                                b.tr(ptlb[:, k * 128:(k + 1) * 128], hlo[:, k * 128:(k + 1) * 128], ident16, [Rhlo, Rc16], rptl)
                            b.op("act", "copy", rptl, [RhloT], out=hloT[:].rearrange("p k t -> p (k t)"), in_=ptlb)
                            if stage == 4.03:
                                raise _Early()
                            pl, rpl = pb1()
                            for k in range(8):
                                hk = h2T[:, k, tt * 128:(tt + 1) * 128]
                                b.mm(pl[:, 0:NE], hk, rwhi[:, k, :], k == 0, False, [Rh2T, Rrwh], rpl)
                                b.mm(pl[:, 0:NE], hk, rwlo[:, k, :], False, False, [Rh2T, Rrwh], rpl)
                                b.mm(pl[:, 0:NE], hloT[:, k, :], rwhi[:, k, :], False, k == 7, [RhloT, Rrwh], rpl)
                            b.op("dve", "tensor_tensor", rpl + [Rrb], [Rlg], out=lg[:], in0=pl[:, 0:NE], in1=rbt[:], op=ALU.add)
                            if stage == 4.04:
                                raise _Early()
                            b.op("dve", "max", [Rlg], [Rm8], out=m8[:], in_=lg[:])
                            b.op("dve", "tensor_scalar", [Rlg, Rm8], [Rmsk], out=msk[:], in0=lg[:], scalar1=m8[:, 3:4], scalar2=None,
                                 op0=ALU.is_ge)
                            b.op("dve", "tensor_scalar", [Rlg, Rm8], [Rex], out=ex[:], in0=lg[:], scalar1=m8[:, 0:1], scalar2=None,
                                 op0=ALU.subtract)
                            b.act(ex[:], ex[:], AF.Exp, [Rex], [Rex])
                            b.op("dve", "tensor_tensor", [Rex, Rmsk], [Rex], out=ex[:], in0=ex[:], in1=msk[:], op=ALU.mult)
                            b.op("dve", "tensor_reduce", [Rex], [Rst2], out=st2[:, 4:5], in_=ex[:], axis=AX.X, op=ALU.add)
                            b.op("dve", "reciprocal", [Rst2], [Rst2], out=st2[:, 5:6], in_=st2[:, 4:5])
                            b.op("dve", "tensor_scalar", [Rex, Rst2], [RG], out=G[:, tt, :], in0=ex[:], scalar1=st2[:, 5:6],
                                 scalar2=None, op0=ALU.mult)
                        if stage == 4.1:
                            raise _Early()
                        b.op("pool", "memset", [], [Racc], acc[:], 0.0)
                        for e in range(NE):
                            wg, Rwg = wg16[nld % 2]
                            wd_, Rwd = wd16[nld % 2]
                            bd_, Rbd = bdt[nld % 2]
                            nld += 1
                            wgv = wgu[e].rearrange("(k p) n -> p k n", p=128)
                            wdv = wdn[e].rearrange("(k p) n -> p k n", p=128)
                            for k in range(8):
                                b.dma("pool", wg[:, k, :], wgv[:, k, :], [], [Rwg], same_gen=(k > 0))
                            for k in range(8):
                                b.dma("pool", wd_[:, k, :], wdv[:, k, :], [], [Rwd], same_gen=(k > 0))
                            b.dma("sp", bd_[:], bdn[e:e + 1, :], [], [Rbd])
                            b.op("act", "copy", [Rbd], [Rbd16], out=bd16[:], in_=bd_[:])
                            if stage == 4.2:
                                raise _Early()
                            for tb in range(QT // 512):
                                for m in range(8):
                                    if stage == 4.3 and m == 1:
                                        raise _Early()
                                    pg, rpg_ = pb1()
                                    pu_, rpu_ = pb1()
                                    for k in range(8):
                                        b.mm(pg, wg[:, k, m * 128:(m + 1) * 128], h2T[:, k, tb * 512:(tb + 1) * 512], k == 0, k == 7,
                                             [Rwg, Rh2T], rpg_)
                                    for k in range(8):
                                        b.mm(pu_, wg[:, k, 1024 + m * 128:1024 + (m + 1) * 128], h2T[:, k, tb * 512:(tb + 1) * 512],
                                             k == 0, k == 7, [Rwg, Rh2T], rpu_)
                                    b.op("act", "copy", rpg_, [Rgsb], out=gsb[:], in_=pg)
                                    b.op("dve", "tensor_scalar", [Rgsb, Rbg], [Rgsb], out=gsb[:], in0=gsb[:], scalar1=bgt[:, e, m:m + 1],
                                         scalar2=7.0, op0=ALU.add, op1=ALU.min)
                                    b.act(sgm[:], gsb[:], AF.Sigmoid, [Rgsb], [Rsgm], scale=1.702)
                                    b.op("act", "copy", rpu_, [Rusb], out=usb[:], in_=pu_)
                                    b.op("dve", "tensor_scalar", [Rusb, Rbg], [Rusb], out=usb[:], in0=usb[:],
                                         scalar1=bgt[:, e, 8 + m:9 + m], scalar2=7.0, op0=ALU.add, op1=ALU.min)
                                    b.op("pool", "tensor_scalar", [Rusb], [Rusb], out=usb[:], in0=usb[:], scalar1=-7.0, scalar2=1.0,
                                         op0=ALU.max, op1=ALU.add)
                                    b.op("pool", "tensor_tensor", [Rgsb, Rsgm], [Rgsb], out=gsb[:], in0=gsb[:], in1=sgm[:], op=ALU.mult)
                                    b.op("pool", "tensor_tensor", [Rgsb, Rusb], [RAT], out=AT[:, m, :], in0=gsb[:], in1=usb[:],
                                         op=ALU.mult)
                                for t4 in range(4):
                                    tt = tb * 4 + t4
                                    py_, rpy_ = pb2()
                                    for half in range(2):
                                        hs = slice(half * 512, (half + 1) * 512)
                                        for k in range(8):
                                            b.mm(py_[:, hs], AT[:, k, t4 * 128:(t4 + 1) * 128], wd_[:, k, hs], k == 0, False,
                                                 [RAT, Rwd], rpy_)
                                        b.mm(py_[:, hs], ones16[0:1, :], bd16[:, hs], False, True, [Rc16, Rbd16], rpy_)
                                    b.op("act", "copy", rpy_, [Rh2f], out=h2f[:, 0:512], in_=py_[:, 0:512])
                                    b.op("act", "copy", rpy_, [Rh2f], out=h2f[:, 512:1024], in_=py_[:, 512:1024])
                                    b.op("dve", "scalar_tensor_tensor", [Rh2f, RG, Racc], [Racc], out=acc[:, tt, :], in0=h2f[:],
                                         scalar=G[:, tt, e:e + 1], in1=acc[:, tt, :], op0=ALU.mult, op1=ALU.add)
                                    if stage == 4.4:
                                        raise _Early()
                        for tt in range(NTQ):
                            tg = q * NTQ + tt
                            b.dma("sp", x1t[:], x1d[tg * 128:(tg + 1) * 128, :], [Rx1], [Rx1t])
                            b.op("dve", "tensor_tensor", [Racc, Rmod], [Rtmp], out=tmp[:], in0=acc[:, tt, :], in1=modb[:, G2, :],
                                 op=ALU.mult)
                            b.op("pool", "tensor_tensor", [Rtmp, Rx1t], [Rxt], out=xt[:], in0=tmp[:], in1=x1t[:], op=ALU.add)
                            b.act(tmp[:], xt[:], AF.Square, [Rxt], [Rtmp, Rst2], accum_out=st2[:, 0:1])
                            b.act(st2[:, 1:2], st2[:, 0:1], AF.Ln, [Rst2], [Rst2], scale=1.0 / D, bias=EPS)
                            b.act(st2[:, 2:3], st2[:, 1:2], AF.Exp, [Rst2], [Rst2], scale=-0.5)
                            b.op("dve", "scalar_tensor_tensor", [Rxt, Rst2, Rfg], [Rh2f], out=h2f[:], in0=xt[:],
                                 scalar=st2[:, 2:3], in1=fgt[:], op0=ALU.mult, op1=ALU.mult)
                            b.op("pool", "tensor_tensor", [Rh2f, Rxt], [Rh2f], out=h2f[:], in0=h2f[:], in1=xt[:], op=ALU.subtract)
                            b.op("dve", "scalar_tensor_tensor", [Rh2f, Rfin, Rxt], [Rtmp], out=tmp[:], in0=h2f[:],
                                 scalar=fint[:, 0:1], in1=xt[:], op0=ALU.mult, op1=ALU.add)
                            b.dma("sp", x_out[xo + tg * 128:xo + (tg + 1) * 128, :], tmp[:], [Rtmp], [Rxo], same_gen=True, final=True)
                    if dbg:
                        b.dma("sp", d_x1[:, :], x1d[:, :], [Rx1], [Rdbg], final=True)
                phE.close()
                segst.close()
                barrier()
        except _Early:
            try:
                phE.close()
            except NameError:
                pass
            phMA.close()
            segst.close()
        S.finalize(top)
    return nc


def make_consts():
    c32 = np.zeros((128, 512), np.float32)
    c32[:, 0:128] = np.eye(128, dtype=np.float32)
    c32[:, 128:256] = 1.0
    k = np.arange(64)
    c32[0:64, 256:320] = (k[:, None] <= k[None, :]).astype(np.float32)
    c32[0:64, 320:384] = np.where(k[None, :] >= k[:, None], BIG, 0.0)
    c32[0:64, 384:448] = np.where(k[None, :] < k[:, None], -BIG, 0.0)
    c16 = np.zeros((128, 256), ml_dtypes.bfloat16)
    c16[:, 0:128] = np.eye(128, dtype=np.float32).astype(ml_dtypes.bfloat16)
    c16[:, 128:256] = np.ones((128, 128), np.float32).astype(ml_dtypes.bfloat16)
    return c32, c16


def layer_weights(inp, l):
    f = lambda a: np.ascontiguousarray(a, dtype=np.float32)
    w_in = inp["w_in"][l]
    o = 0
    qkv = w_in[:, o:o + 3072]; o += 3072
    gz = w_in[:, o:o + 1024]; o += 1024
    gb = w_in[:, o:o + 8]; o += 8
    ga = w_in[:, o:o + 8]; o += 8
    sz = w_in[:, o:o + 1024]; o += 1024
    xbc = w_in[:, o:o + 1536]; o += 1536
    sdt = w_in[:, o:o + 16]
    w_cm = np.concatenate([qkv, xbc], axis=1)
    w_tm = np.concatenate([gz, sz, gb, ga, sdt], axis=1)
    cw = np.concatenate([inp["gdn_conv_w"][l], inp["ssd_conv_w"][l]], axis=1)
    conv_w = cw.reshape(4, 36, 128).transpose(2, 1, 0)
    cbias = np.concatenate([np.zeros(3072, np.float32), inp["ssd_conv_b"][l]])
    conv_b = cbias.reshape(36, 128).T
    hpv = np.concatenate([inp["gdn_a_log"][l], inp["gdn_dt_bias"][l], inp["ssd_a_log"][l], inp["ssd_dt_bias"][l],
                          inp["ssd_d"][l]])
    hp = np.tile(hpv[None, :], (64, 1))
    ng = np.concatenate([np.tile(inp["gdn_norm_g"][l], 8), inp["ssd_norm_g"][l]])
    normg = np.tile(ng[None, :], (64, 1))
    bgu = inp["moe_b_gu"][l].reshape(32, 16, 128).transpose(2, 0, 1)
    rep = lambda v: np.tile(np.asarray(v)[None, :], (128, 1))
    return dict(
        ada_w=f(inp["ada_w"][l]), ada_b=f(inp["ada_b"][l][None, :]), n1g=f(rep(inp["norm1_g"][l])),
        n2g=f(rep(inp["norm2_g"][l])), fg=f(rep(inp["final_g"])), w_cm=f(w_cm), w_tm=f(w_tm), conv_w=f(conv_w),
        conv_b=f(conv_b), hp=f(hp), normg=f(normg), w_out=f(inp["w_out"][l]), r_w=f(inp["router_w"][l]),
        r_b=f(rep(inp["router_b"][l])), wgu=f(inp["moe_w_gu"][l]), bgu=f(bgu), wdn=f(inp["moe_w_down"][l]),
        bdn=f(inp["moe_b_down"][l]))


def zero_state():
    return dict(st_tail=np.zeros((4608, 3), np.float32), st_S=np.zeros((128, 8, 128), np.float32),
                st_h=np.zeros((128, 2, 512), np.float32))


def kernel(**inputs):
    inp = {k: np.asarray(v) for k, v in inputs.items()}
    T, NSEG = 4096, 4
    nc = build_layer(T, NE=32, NSEG=NSEG)
    c32, c16 = make_consts()
    cores = [0, 1]
    x = np.ascontiguousarray(inp["x"], dtype=np.float32)
    cv = [np.ascontiguousarray(inp["c"][b].reshape(8, 128).T, dtype=np.float32) for b in range(4)]
    cur = [x[0:2].reshape(2 * 8192, D), x[2:4].reshape(2 * 8192, D)]
    for l in range(2):
        W = layer_weights(inp, l)
        fin = np.full((128, 1), 1.0 if l == 1 else 0.0, np.float32)
        maps = []
        for ci in range(2):
            m = dict(W)
            m.update(zero_state())
            m["x"] = np.ascontiguousarray(cur[ci])
            b0, b1 = 2 * ci, 2 * ci + 1
            m["cvec"] = np.stack([cv[b0], cv[b0], cv[b1], cv[b1]])
            m["fin"] = fin
            m["c32"] = c32
            m["c16"] = c16
            maps.append(m)
        res = run_bass_kernel_spmd(nc, maps, core_ids=cores).results
        cur = [np.asarray(res[ci]["x_out"]) for ci in range(2)]
    out = np.concatenate([cur[0].reshape(2, 8192, D), cur[1].reshape(2, 8192, D)], axis=0)
    return np.ascontiguousarray(out, dtype=np.float32)
```
